# Optimizing a Trainium2 kernel written in Bass

```python
import jax, jax.numpy as jnp
from jax import lax
import numpy as np

D_MODEL = 2048
BATCH = 4
SEQ = 4096
DEPTH = 2

N_HEADS = D_MODEL // 128
HEAD_DIM = 64
N_KV_HEADS = N_HEADS // 4
GQA_GROUP = N_HEADS // N_KV_HEADS
D_ATTN = N_HEADS * HEAD_DIM
D_KV = N_KV_HEADS * HEAD_DIM
WINDOW = 128
BLOCK = 128
D_CONV = D_MODEL // 2
CONV_WIDTH = 3
D_FF = ((8 * D_MODEL // 3 + 255) // 256) * 256
N_EXPERTS = 8
TOP_K = 2
MOE_D_FF = 7 * D_MODEL // 2
N_DENSE = (DEPTH + 1) // 2
N_MOE = DEPTH // 2
N_MOD = 6
EPS = 1e-6

IN_WIDTHS = [D_ATTN, D_KV, D_KV, D_CONV, D_CONV, D_CONV, D_MODEL, D_MODEL]
IN_SPLITS = [int(s) for s in np.cumsum(IN_WIDTHS)[:-1]]
D_IN = int(sum(IN_WIDTHS))

kernel_name = "hybrid_swa_shortconv_moe_adaln"


def rms_norm(x, gain):
    xf = x.astype(jnp.float32)
    y = xf * lax.rsqrt(jnp.mean(xf * xf, axis=-1, keepdims=True) + EPS)
    return (y * gain.astype(jnp.float32)).astype(x.dtype)


def alibi_slopes(n_heads):
    return 2.0 ** (-8.0 * jnp.arange(1, n_heads + 1, dtype=jnp.float32) / n_heads)


def sliding_window_attention(q, k, v, sinks):
    b, s = q.shape[0], q.shape[1]
    nb = s // BLOCK
    qb = q.reshape(b, nb, BLOCK, N_KV_HEADS, GQA_GROUP, HEAD_DIM)

    def band(t):
        tb = t.reshape(b, nb, BLOCK, N_KV_HEADS, HEAD_DIM)
        prev = jnp.pad(tb[:, :-1], ((0, 0), (1, 0), (0, 0), (0, 0), (0, 0)))
        return jnp.concatenate([prev, tb], axis=2)

    kw, vw = band(k), band(v)
    scores = jnp.einsum('bnqkgd,bnskd->bnkgqs', qb, kw,
                        preferred_element_type=jnp.float32) * (HEAD_DIM ** -0.5)
    q_rel = jnp.arange(BLOCK) + BLOCK
    k_rel = jnp.arange(2 * BLOCK)
    dist = q_rel[:, None] - k_rel[None, :]
    key_abs = (jnp.arange(nb) * BLOCK - BLOCK)[:, None] + k_rel[None, :]
    valid = ((dist >= 0) & (dist < WINDOW))[None] & (key_abs >= 0)[:, None, :]
    slopes = alibi_slopes(N_HEADS).reshape(N_KV_HEADS, GQA_GROUP)
    logits = scores - slopes[:, :, None, None] * dist.astype(jnp.float32)
    logits = jnp.where(valid[None, :, None, None], logits, -jnp.inf)
    sink = jnp.broadcast_to(sinks.astype(jnp.float32).reshape(N_KV_HEADS, GQA_GROUP, 1, 1),
                            logits.shape[:-1] + (1,))
    probs = jax.nn.softmax(jnp.concatenate([logits, sink], axis=-1), axis=-1)[..., :-1]
    out = jnp.einsum('bnkgqs,bnskd->bnqkgd', probs.astype(v.dtype), vw)
    return out.reshape(b, s, D_ATTN)


def short_conv(u, w):
    return lax.conv_general_dilated(u, w[:, None, :], window_strides=(1,),
                                    padding=[(CONV_WIDTH - 1, 0)],
                                    dimension_numbers=('NWC', 'WIO', 'NWC'),
                                    feature_group_count=D_CONV)


def swiglu(h, w_gate_up, w_down):
    g, u = jnp.split(h @ w_gate_up, 2, axis=-1)
    return (jax.nn.silu(g) * u) @ w_down


def moe_swiglu(h, w_router, w_gate_up, w_down):
    t = h.reshape(-1, D_MODEL)
    logits = (t @ w_router).astype(jnp.float32)
    top_v, top_i = lax.top_k(logits, TOP_K)
    top_w = jax.nn.softmax(top_v, axis=-1)
    gates = jnp.sum(jax.nn.one_hot(top_i, N_EXPERTS, dtype=jnp.float32) * top_w[..., None], axis=1)
    out = jnp.zeros_like(t)
    for e in range(N_EXPERTS):
        out = out + gates[:, e:e + 1].astype(t.dtype) * swiglu(t, w_gate_up[e], w_down[e])
    return out.reshape(h.shape)


def setup_inputs(seed: int = 0) -> dict:
    key = jax.random.key(seed)
    ks = jax.random.split(key, 20)

    def nrm(k, shape, scale):
        return jax.random.normal(k, shape, jnp.float32) * scale

    return {
        "x": nrm(ks[0], (BATCH, SEQ, D_MODEL), 1.0),
        "c": nrm(ks[1], (BATCH, D_MODEL), 1.0),
        "ada_w": nrm(ks[2], (DEPTH, D_MODEL, N_MOD * D_MODEL), 0.5 * D_MODEL ** -0.5),
        "ada_b": nrm(ks[3], (DEPTH, N_MOD * D_MODEL), 0.02),
        "norm_mix": 1.0 + nrm(ks[4], (DEPTH, D_MODEL), 0.05),
        "w_in": nrm(ks[5], (DEPTH, D_MODEL, D_IN), D_MODEL ** -0.5),
        "q_norm": 1.0 + nrm(ks[6], (DEPTH, HEAD_DIM), 0.05),
        "k_norm": 1.0 + nrm(ks[7], (DEPTH, HEAD_DIM), 0.05),
        "attn_sinks": nrm(ks[8], (DEPTH, N_HEADS), 0.5),
        "conv_w": nrm(ks[9], (DEPTH, CONV_WIDTH, D_CONV), CONV_WIDTH ** -0.5),
        "w_attn_branch": nrm(ks[10], (DEPTH, D_ATTN, D_MODEL), D_ATTN ** -0.5),
        "w_conv_branch": nrm(ks[11], (DEPTH, D_CONV, D_MODEL), D_CONV ** -0.5),
        "w_out": nrm(ks[12], (DEPTH, D_MODEL, D_MODEL), D_MODEL ** -0.5),
        "norm_ffn": 1.0 + nrm(ks[13], (DEPTH, D_MODEL), 0.05),
        "ffn_w_gate_up": nrm(ks[14], (N_DENSE, D_MODEL, 2 * D_FF), D_MODEL ** -0.5),
        "ffn_w_down": nrm(ks[15], (N_DENSE, D_FF, D_MODEL), D_FF ** -0.5),
        "moe_w_router": nrm(ks[16], (N_MOE, D_MODEL, N_EXPERTS), D_MODEL ** -0.5),
        "moe_w_gate_up": nrm(ks[17], (N_MOE, N_EXPERTS, D_MODEL, 2 * MOE_D_FF), D_MODEL ** -0.5),
        "moe_w_down": nrm(ks[18], (N_MOE, N_EXPERTS, MOE_D_FF, D_MODEL), MOE_D_FF ** -0.5),
    }


def reference(x, c, ada_w, ada_b, norm_mix, w_in, q_norm, k_norm, attn_sinks, conv_w,
              w_attn_branch, w_conv_branch, w_out, norm_ffn, ffn_w_gate_up, ffn_w_down,
              moe_w_router, moe_w_gate_up, moe_w_down):
    b, s = x.shape[0], x.shape[1]
    for l in range(DEPTH):
        mod = jax.nn.silu(c) @ ada_w[l] + ada_b[l]
        shift_m, scale_m, gate_m, shift_f, scale_f, gate_f = jnp.split(mod[:, None, :], N_MOD, axis=-1)

        h = rms_norm(x, norm_mix[l]) * (1.0 + scale_m) + shift_m
        q, k, v, conv_b, conv_c, conv_x, g_attn, g_conv = jnp.split(h @ w_in[l], IN_SPLITS, axis=-1)
        q = rms_norm(q.reshape(b, s, N_HEADS, HEAD_DIM), q_norm[l])
        k = rms_norm(k.reshape(b, s, N_KV_HEADS, HEAD_DIM), k_norm[l])
        v = v.reshape(b, s, N_KV_HEADS, HEAD_DIM)
        attn = sliding_window_attention(q, k, v, attn_sinks[l])
        conv = conv_b * short_conv(conv_c * conv_x, conv_w[l])
        merged = (jax.nn.sigmoid(g_attn) * (attn @ w_attn_branch[l])
                  + jax.nn.sigmoid(g_conv) * (conv @ w_conv_branch[l]))
        x = x + gate_m * (merged @ w_out[l])

        h = rms_norm(x, norm_ffn[l]) * (1.0 + scale_f) + shift_f
        if l % 2 == 0:
            f = swiglu(h, ffn_w_gate_up[l // 2], ffn_w_down[l // 2])
        else:
            f = moe_swiglu(h, moe_w_router[l // 2], moe_w_gate_up[l // 2], moe_w_down[l // 2])
        x = x + gate_f * f
    return x
```

```python
import contextlib
import numpy as np
import concourse.bass as bass
import concourse.mybir as mybir
from concourse.bass_utils import run_bass_kernel_spmd

F32 = mybir.dt.float32
BF16 = mybir.dt.bfloat16
AF = mybir.ActivationFunctionType
ALU = mybir.AluOpType
AX = mybir.AxisListType

D = 2048
KC = 16
NBLK = 18
NTOK = NBLK * 128
DEPTH = 2
D_IN = 8704
D_FF = 5632
MOE_FF = 7168
NEXP = 8
EPS = 1e-6
NEG = -30000.0
QO, KO, VO, BO, CO, XO, GAO, GCO = 0, 1024, 1280, 1536, 2560, 3584, 4608, 6656


class Q:
    def __init__(self, eng, sem_h, sid):
        self.e = eng
        self.sem = sem_h
        self.sid = sid
        self.n = 0
        self.seen = {}

    def wait_ev(self, sid, sem_h, val):
        if self.seen.get(sid, 0) >= val:
            return
        self.e.wait_ge(sem_h, val)
        self.seen[sid] = val


class Sched:
    def __init__(self, nc, es):
        self.nc = nc
        self.es = es
        self.sems = {}
        self.nsem = 0
        self.res = {}
        self.pe = self.newq(nc.tensor, "pe")
        self.act = self.newq(nc.scalar, "act")
        self.dve = self.newq(nc.vector, "dve")
        self.sp = self.newq(nc.sync, "sp")
        self.pool = self.newq(nc.gpsimd, "pool")
        self.dsems = [self.newsem(f"spd{i}") for i in range(8)]
        self.dcnt = [0] * 8
        self.di = 0

    def newsem(self, name):
        h = self.es.enter_context(self.nc.semaphore(name))
        self.nsem += 1
        self.sems[self.nsem] = h
        return self.nsem

    def newq(self, eng, name):
        sid = self.newsem(name)
        return Q(eng, self.sems[sid], sid)

    def _deps(self, reads, writes):
        deps = {}

        def add(ev):
            if ev is None:
                return
            sid, val = ev
            if deps.get(sid, 0) < val:
                deps[sid] = val

        for k in reads:
            r = self.res.get(k)
            if r:
                add(r["w"])
        for k in writes:
            r = self.res.get(k)
            if r:
                add(r["w"])
                for sid, val in r["r"].items():
                    add((sid, val))
        return deps

    def _record(self, ev, reads, writes):
        sid, val = ev
        for k in reads:
            r = self.res.setdefault(k, {"w": None, "r": {}})
            if r["r"].get(sid, 0) < val:
                r["r"][sid] = val
        for k in writes:
            self.res[k] = {"w": ev, "r": {}}

    def op(self, q, fn, reads=(), writes=()):
        psr = [k for k in reads if isinstance(k, tuple) and k[0] == "ps"]
        if psr:
            reads = [k for k in reads if not (isinstance(k, tuple) and k[0] == "ps")]
            writes = list(writes) + psr
        for sid, val in self._deps(reads, writes).items():
            q.wait_ev(sid, self.sems[sid], val)
        inst = fn()
        q.n += 1
        inst.then_inc(q.sem, 1)
        ev = (q.sid, q.n)
        self._record(ev, reads, writes)
        return ev

    def dma(self, out, in_, reads=(), writes=()):
        q = self.sp
        k = self.di % 8
        self.di += 1
        sid = self.dsems[k]
        if self.dcnt[k]:
            q.wait_ev(sid, self.sems[sid], 16 * self.dcnt[k])
        for s2, val in self._deps(reads, writes).items():
            q.wait_ev(s2, self.sems[s2], val)
        q.e.dma_start(out=out, in_=in_).then_inc(self.sems[sid], 16)
        self.dcnt[k] += 1
        ev = (sid, 16 * self.dcnt[k])
        self._record(ev, reads, writes)
        return ev

    def barrier(self):
        evs = {}
        for q in (self.pe, self.act, self.dve):
            evs[q.sid] = q.n
        for k in range(8):
            if self.dcnt[k]:
                evs[self.dsems[k]] = 16 * self.dcnt[k]
        for q in (self.pe, self.act, self.dve, self.sp):
            for sid, val in evs.items():
                if val:
                    q.wait_ev(sid, self.sems[sid], val)


class Ring:
    def __init__(self, S, slots):
        self.S = S
        self.slots = slots
        self.sids = [S.newsem(f"ring{i}") for i in range(len(slots))]
        self.cnt = [0] * len(slots)
        self.i = 0

    def load(self, pairs_fn):
        S = self.S
        k = self.i % len(self.slots)
        self.i += 1
        q = S.pool
        key = ("slot", k)
        for sid, val in S._deps((), (key,)).items():
            q.wait_ev(sid, S.sems[sid], val)
        for out, in_ in pairs_fn(self.slots[k]):
            q.e.dma_start(out=out, in_=in_).then_inc(S.sems[self.sids[k]], 16)
            self.cnt[k] += 1
        S._record((self.sids[k], 16 * self.cnt[k]), (), (key,))
        return self.slots[k], key


def build(dbg=None):
    dbg = dbg or {}
    nc = bass.Bass("TRN2", target_bir_lowering=False)
    es = contextlib.ExitStack()

    def din(name, shape, dt=F32):
        if name in dbg.get("small", ()):
            shape = [1] * len(shape)
        return nc.dram_tensor(name, list(shape), dt, kind="ExternalInput").ap()

    xh = din("xh", [NTOK, D])
    ccol_d = din("ccol", [128, KC])
    flag_d = din("flag", [128, 2])
    ident_d = din("ident", [128, 128])
    bcur_d = din("biascur", [128, 2048])
    bprev_d = din("biasprev", [128, 2048])
    ada_w = din("ada_w", [DEPTH, D, 6 * D])
    ada_b = din("ada_b", [DEPTH, 6 * D])
    norm_mix = din("norm_mix", [DEPTH, D])
    w_in = din("w_in", [DEPTH, D, D_IN])
    q_norm = din("q_norm", [DEPTH, 64])
    k_norm = din("k_norm", [DEPTH, 64])
    attn_sinks = din("attn_sinks", [DEPTH, 16])
    conv_w = din("conv_w", [DEPTH, 3, 1024])
    w_ab = din("w_attn_branch", [DEPTH, 1024, D])
    w_cb = din("w_conv_branch", [DEPTH, 1024, D])
    w_out = din("w_out", [DEPTH, D, D])
    norm_ffn = din("norm_ffn", [DEPTH, D])
    ffn_gu = din("ffn_w_gate_up", [1, D, 2 * D_FF])
    ffn_dn = din("ffn_w_down", [1, D_FF, D])
    moe_r = din("moe_w_router", [1, D, NEXP])
    moe_gu = din("moe_w_gate_up", [1, NEXP, D, 2 * MOE_FF])
    moe_dn = din("moe_w_down", [1, NEXP, MOE_FF, D])

    y = nc.dram_tensor("y", [2048, D], F32, kind="ExternalOutput").ap()
    xs_kind = "ExternalOutput" if dbg.get("xs_out") else "Internal"
    xs = nc.dram_tensor("xs", [NTOK, D], F32, kind=xs_kind).ap()
    mod_d = nc.dram_tensor("mod_d", [DEPTH, 6 * D], F32, kind="Internal").ap()
    hT_d = nc.dram_tensor("hT_d", [KC, 128, NTOK], BF16, kind="Internal").ap()
    aT_d = nc.dram_tensor("aT_d", [8, 128, NTOK], BF16, kind="Internal").ap()
    cT_d = nc.dram_tensor("cT_d", [8, 128, NTOK], BF16, kind="Internal").ap()

    es.enter_context(nc.allow_low_precision("bf16 matmul operands, fp32 accumulate"))
    es.enter_context(nc.allow_non_contiguous_dma("small strided parameter loads"))

    S = Sched(nc, es)
    pe, act, dve = S.pe, S.act, S.dve
    V, A, T = nc.vector, nc.scalar, nc.tensor
    sb_i = [0]

    def sb(name, shape, dt=F32, stack=None):
        sb_i[0] += 1
        return (stack or es).enter_context(nc.sbuf_tensor(f"{name}_s{sb_i[0]}", list(shape), dt))

    NSLOT = 4
    ring = Ring(S, [sb(f"wslot{i}", [128, KC, 512], BF16) for i in range(NSLOT)])
    pst = es.enter_context(nc.psum_tensor("ps", [128, 8, 512], F32))
    ps_i = [0]

    def bank_alloc():
        k = ps_i[0] % 8
        ps_i[0] += 1
        return pst[:, k, :], ("ps", k)

    def mm(out, pairs, reads, writes):
        n = len(pairs)

        def fn():
            inst = None
            for i, (l_, r_) in enumerate(pairs):
                inst = T.matmul(out, lhsT=l_, rhs=r_, start=(i == 0), stop=(i == n - 1))
            return inst
        return S.op(pe, fn, reads, writes)

    ident = sb("ident", [128, 128])
    flag = sb("flag", [128, 2])
    ccol = sb("ccolsb", [128, KC])
    siluc = sb("siluc", [128, KC], BF16)
    modT = sb("modT", [128, DEPTH, 6, KC])
    normT = sb("normT", [128, DEPTH, 2, KC])
    gcol = sb("gcol", [128, DEPTH, 2, KC])
    qkg = sb("qkg", [128, DEPTH, 2])
    esink = sb("esink", [128, DEPTH, 16])
    convw = sb("convw", [128, DEPTH, 3, 8])
    blockones = sb("blockones", [128, 128], BF16)
    ones_bf = sb("ones_bf", [128, 128], BF16)
    wr_sb = sb("wr_sb", [128, KC, NEXP], BF16)
    stgB = sb("stgB", [56, DEPTH, 128])
    eps_t = sb("eps_t", [128, 1])

    S.dma(ident[:], ident_d, writes=["ident"])
    S.dma(flag[:], flag_d, writes=["flag"])
    S.dma(ccol[:], ccol_d, writes=["ccol"])
    for l in range(DEPTH):
        S.dma(stgB[0:16, l, :], norm_mix[l].rearrange("(k p) -> k p", p=128), writes=[("stgB", l, 0)])
        S.dma(stgB[16:32, l, :], norm_ffn[l].rearrange("(k p) -> k p", p=128), writes=[("stgB", l, 1)])
        S.dma(stgB[32:56, l, :], conv_w[l].rearrange("j (c p) -> (j c) p", p=128), writes=[("stgB", l, 2)])
        for half in range(2):
            S.dma(qkg[half * 64:(half + 1) * 64, l, 0:1], q_norm[l].rearrange("(p o) -> p o", o=1),
                  writes=[("qkg", l, half, 0)])
            S.dma(qkg[half * 64:(half + 1) * 64, l, 1:2], k_norm[l].rearrange("(p o) -> p o", o=1),
                  writes=[("qkg", l, half, 1)])
        S.dma(esink[:, l, :], attn_sinks[l].partition_broadcast(128), writes=[("esink", l)])
    wr_sid = S.newsem("wr")
    nc.gpsimd.dma_start(out=wr_sb[:], in_=moe_r[0].rearrange("(k p) e -> p k e", p=128)).then_inc(S.sems[wr_sid], 16)
    S._record((wr_sid, 16), (), ["wr"])

    S.op(dve, lambda: V.memset(blockones[:], 0.0), writes=["bo"])
    S.op(dve, lambda: V.memset(blockones[0:64, 0:64], 1.0), writes=["bo"])
    S.op(dve, lambda: V.memset(blockones[64:128, 64:128], 1.0), writes=["bo"])
    S.op(dve, lambda: V.memset(eps_t[:], EPS), writes=["eps"])
    S.op(dve, lambda: V.memset(ones_bf[:], 1.0), writes=["ones"])
    S.op(act, lambda: A.activation(out=siluc[:], in_=ccol[:], func=AF.Silu), reads=["ccol"], writes=["siluc"])
    for l in range(DEPTH):
        S.op(act, lambda: A.activation(out=esink[:, l, :], in_=esink[:, l, :], func=AF.Exp), writes=[("esink", l)])
        S.op(act, lambda: A.mul(out=qkg[:, l, 0:1], in_=qkg[:, l, 0:1], mul=0.125),
             writes=[("qkg", l, 0, 0), ("qkg", l, 1, 0)])
    for l in range(DEPTH):
        S.op(pe, lambda: T.transpose(out=pst[:, 7, l * 64:l * 64 + 56], in_=stgB[0:56, l, :], identity=ident[0:56, 0:56]),
             reads=["ident", ("stgB", l, 0), ("stgB", l, 1), ("stgB", l, 2)], writes=[("ps", 7)])
    for l in range(DEPTH):
        S.op(dve, lambda: V.tensor_copy(out=normT[:, l, :, :],
                                        in_=pst[:, 7, l * 64:l * 64 + 32].rearrange("p (a k) -> p a k", k=KC)),
             reads=[("ps", 7)], writes=[("normT", l)])
        S.op(dve, lambda: V.tensor_copy(out=convw[:, l, :, :],
                                        in_=pst[:, 7, l * 64 + 32:l * 64 + 56].rearrange("p (j c) -> p j c", c=8)),
             reads=[("ps", 7)], writes=[("convw", l)])

    def CK(l):
        return [("qkg", l, 0, 0), ("qkg", l, 1, 0), ("qkg", l, 0, 1), ("qkg", l, 1, 1), ("esink", l), ("convw", l),
                ("mod", l), "flag", "eps", "bo", "ones", "ident"]

    with contextlib.ExitStack() as pa:
        abuf = [sb(f"abuf{i}", [1, 512], stack=pa) for i in range(2)]
        rbuf = [sb(f"rbuf{i}", [1, 512], stack=pa) for i in range(2)]
        stgA = sb("stgA", [96, DEPTH, 128], stack=pa)
        for l in range(DEPTH if not dbg.get("skipA") else 0):
            for n in range(24):
                slot, skey = ring.load(lambda s: [
                    (s[:, :, :], ada_w[l, :, n * 512:(n + 1) * 512].rearrange("(k p) n -> p k n", p=128))])
                bank, bkey = bank_alloc()
                mm(bank[0:1, :], [(siluc[:, k:k + 1], slot[:, k, :]) for k in range(KC)],
                   reads=[skey, "siluc"], writes=[bkey])
                i2 = n % 2
                S.dma(abuf[i2][0:1, :], ada_b[l, n * 512:(n + 1) * 512].rearrange("(o n) -> o n", o=1),
                      writes=[("abuf", i2)])
                S.op(dve, lambda: V.tensor_tensor(out=rbuf[i2][0:1, :], in0=bank[0:1, :], in1=abuf[i2][0:1, :], op=ALU.add),
                     reads=[bkey, ("abuf", i2)], writes=[("rbuf", i2)])
                S.dma(mod_d[l, n * 512:(n + 1) * 512].rearrange("(o n) -> o n", o=1), rbuf[i2][0:1, :],
                      reads=[("rbuf", i2)], writes=[("mod_d", l, n)])
        if dbg.get("skipA"):
            for l in range(DEPTH):
                S.op(dve, lambda: V.memset(stgA[:, l, :], 0.25), writes=[("stgA", l)])
        for l in range(DEPTH if not dbg.get("skipA") else 0):
            S.dma(stgA[:, l, :], mod_d[l].rearrange("(r p) -> r p", p=128),
                  reads=[("mod_d", l, n) for n in range(24)], writes=[("stgA", l)])
        for l in range(DEPTH):
            bank, bkey = bank_alloc()
            S.op(pe, lambda: T.transpose(out=bank[:, 0:96], in_=stgA[0:96, l, :], identity=ident[0:96, 0:96]),
                 reads=["ident", ("stgA", l)], writes=[bkey])
            S.op(dve, lambda: V.tensor_copy(out=modT[:, l, :, :], in_=bank[:, 0:96].rearrange("p (s k) -> p s k", k=KC)),
                 reads=[bkey], writes=[("mod", l)])
            for i, s_ in enumerate((1, 4)):
                S.op(dve, lambda: V.scalar_tensor_tensor(out=gcol[:, l, i, :], in0=modT[:, l, s_, :], scalar=1.0,
                                                         in1=normT[:, l, i, :], op0=ALU.add, op1=ALU.mult),
                     reads=[("normT", l)], writes=[("mod", l)])
        S.barrier()

    if dbg.get("stop") == "A":
        S.dma(y[0:128, 0:DEPTH * 6 * KC], modT[:].rearrange("p l s k -> p (l s k)"), reads=[("mod", 0), ("mod", 1)])
        S.barrier()
        es.close()
        return nc

    def tiles_from(b_first):
        t = []
        if b_first < 2:
            t.append(list(range(b_first, 2)))
        t += [list(range(2 + 4 * i, 6 + 4 * i)) for i in range(4)]
        return t

    def norm_transpose(l, which, xb_ap, xkeys, hT, hkey, bi, junk, ss):
        S.op(dve, lambda: V.memset(ss[:, 0:1], 0.0), writes=["ss"])
        S.op(act, lambda: A.activation(out=junk[:], in_=xb_ap, func=AF.Square, accum_out=ss[:, 0:1]),
             reads=xkeys, writes=["xn", "ss"])
        S.op(act, lambda: A.activation(out=ss[:, 1:2], in_=ss[:, 0:1], func=AF.Ln, bias=eps_t[:, 0:1], scale=1.0 / D),
             reads=["eps"], writes=["ss"])
        S.op(act, lambda: A.activation(out=ss[:, 2:3], in_=ss[:, 1:2], func=AF.Exp, scale=-0.5), writes=["ss"])
        S.op(act, lambda: A.activation(out=junk[:], in_=xb_ap, func=AF.Copy, scale=ss[:, 2:3]),
             reads=xkeys + ["ss"], writes=["xn"])
        sh = 3 * which
        for kg in range(4):
            bank, bkey = bank_alloc()

            def tr():
                inst = None
                for j in range(4):
                    k = kg * 4 + j
                    inst = T.transpose(out=bank[:, j * 128:(j + 1) * 128], in_=junk[:, k * 128:(k + 1) * 128],
                                       identity=ident[:])
                return inst
            S.op(pe, tr, reads=["xn", "ident"], writes=[bkey])
            for j in range(4):
                k = kg * 4 + j
                o = hT[:, k, bi * 128:(bi + 1) * 128]
                i_ = bank[:, j * 128:(j + 1) * 128]
                S.op(dve, lambda: V.tensor_scalar(out=o, in0=i_, scalar1=gcol[:, l, which, k:k + 1],
                                                  scalar2=modT[:, l, sh, k:k + 1], op0=ALU.mult, op1=ALU.add),
                     reads=[bkey, ("mod", l)], writes=[(hkey, bi, k)])

    def hkeys(hkey, nb, bis=None):
        bis = range(nb) if bis is None else bis
        return [(hkey, bi, k) for bi in bis for k in range(KC)]

    for l in range(DEPTH):
        b0 = l
        xsrc = xh if l == 0 else xs
        ck = CK(l)

        with contextlib.ExitStack() as pm:
            bcur = sb("bcur", [128, 2048], stack=pm)
            bprev = sb("bprev", [128, 2048], stack=pm)
            xblk = sb("xblk", [128, D], stack=pm)
            xn = sb("xn", [128, D], stack=pm)
            ss = sb("ss", [128, 4], stack=pm)
            hT = sb("hT", [128, KC, 512], BF16, stack=pm)
            qT = sb("qT", [128, 8, 512], BF16, stack=pm)
            kT = [[sb(f"kT{i}_{par}", [128, 4, 4, 128], BF16, stack=pm) for par in range(2)] for i in range(2)]
            Vd = [sb(f"Vd{i}", [128, 4, 4, 128], BF16, stack=pm) for i in range(2)]
            sqt = [sb(f"sqt{i}", [128, 512], BF16, stack=pm) for i in range(2)]
            lnt = [sb(f"lnt{i}", [128, 512], stack=pm) for i in range(2)]
            rst = [sb(f"rst{i}", [128, 512], stack=pm) for i in range(2)]
            tS = [sb(f"tS{i}", [128, 512], stack=pm) for i in range(2)]
            PT = [sb(f"PT{i}", [128, 512], BF16, stack=pm) for i in range(4)]
            rc = [sb(f"rc{i}", [128, 512], stack=pm) for i in range(2)]
            aT = sb("aT", [128, 8, 512], BF16, stack=pm)
            Bt = sb("Bt", [128, 8, 512], BF16, stack=pm)
            Ct = [sb(f"Ct{i}", [128, 512], stack=pm) for i in range(2)]
            ut = [sb(f"ut{i}", [128, 516], stack=pm) for i in range(2)]
            yt = [sb(f"yt{i}", [128, 512], stack=pm) for i in range(2)]
            ucar = [sb(f"ucar{i}", [128, 8, 2], stack=pm) for i in range(2)]
            cT = sb("cT", [128, 8, 512], BF16, stack=pm)

            S.dma(bcur[:], bcur_d, writes=["bcur"])
            S.dma(bprev[:], bprev_d, writes=["bprev"])
            for i in range(2):
                for par in range(2):
                    S.op(dve, lambda: V.memset(kT[i][par][:], 0.0), writes=[("kT", i, par, g) for g in range(4)])
            S.op(dve, lambda: V.memset(ucar[1][:], 0.0), writes=[("ucar", 1, c) for c in range(8)])

            tl = tiles_from(b0)
            for ti, bl in enumerate(tl):
                if ti >= dbg.get('m1_tiles', 99):
                    break
                nb = len(bl)
                N = nb * 128
                tok0 = bl[0] * 128
                cb, pb = ti % 2, (ti + 1) % 2
                nb_prev = len(tl[ti - 1]) if ti > 0 else 0
                hk = hkeys("hT", nb)

                for bi, b in enumerate(bl):
                    S.dma(xblk[:], xsrc[b * 128:(b + 1) * 128, :], reads=[("xs", b)], writes=["xblk"])
                    norm_transpose(l, 0, xblk[:], ["xblk"], hT, "hT", bi, xn, ss)
                S.dma(hT_d[:, :, tok0:tok0 + N].rearrange("k p t -> p k t"), hT[:, :, 0:N], reads=hk,
                      writes=[("hTd", b) for b in bl])

                if dbg.get('m1_steps', 9) < 3:
                    continue
                def qk_stage2(it):
                    i2 = it["i2"]
                    bank, bkey = it["bank"], it["bkey"]
                    bank2, bkey2 = bank_alloc()
                    mm(bank2[:, 0:N], [(blockones[:], sqt[i2][:, 0:N])], reads=[("sqt", i2), "bo"], writes=[bkey2])
                    S.op(act, lambda: A.activation(out=lnt[i2][:, 0:N], in_=bank2[:, 0:N], func=AF.Ln,
                                                   bias=eps_t[:, 0:1], scale=1.0 / 64),
                         reads=[bkey2, "eps"], writes=[("lnt", i2)])
                    S.op(act, lambda: A.activation(out=rst[i2][:, 0:N], in_=lnt[i2][:, 0:N], func=AF.Exp, scale=-0.5),
                         reads=[("lnt", i2)], writes=[("rst", i2)])
                    if it["kind"] == "q":
                        c = it["c"]
                        S.op(dve, lambda: V.scalar_tensor_tensor(out=qT[:, c, 0:N], in0=bank[:, 0:N],
                                                                 scalar=qkg[:, l, 0:1], in1=rst[i2][:, 0:N],
                                                                 op0=ALU.mult, op1=ALU.mult),
                             reads=[bkey, ("rst", i2)] + ck, writes=[("qT", c)])
                    else:
                        for hh in range(2):
                            g = 2 * it["c"] + hh
                            src = slice(hh * 64, hh * 64 + 64)
                            for par in range(2):
                                dst = slice(par * 64, par * 64 + 64)
                                S.op(dve, lambda: V.scalar_tensor_tensor(
                                    out=kT[cb][par][dst, g, 0:nb, :],
                                    in0=bank[src, 0:N].rearrange("p (b t) -> p b t", t=128),
                                    scalar=qkg[src, l, 1:2],
                                    in1=rst[i2][src, 0:N].rearrange("p (b t) -> p b t", t=128),
                                    op0=ALU.mult, op1=ALU.mult),
                                    reads=[bkey, ("rst", i2)] + ck, writes=[("kT", cb, par, g)])

                pend = []
                si = 0
                units = [(QO, [("q", 0), ("q", 1), ("q", 2), ("q", 3)]),
                         (QO + 512, [("q", 4), ("q", 5), ("q", 6), ("q", 7)]),
                         (KO, [("k", 0), ("k", 1)])]
                for col0, chunks in units[:dbg.get('units', 3)]:
                    slot, skey = ring.load(lambda s: [
                        (s[:, :, :], w_in[l, :, col0:col0 + 512].rearrange("(k p) n -> p k n", p=128))])
                    for ci, (kind, c) in enumerate(chunks):
                        bank, bkey = bank_alloc()
                        mm(bank[:, 0:N], [(slot[:, k, ci * 128:(ci + 1) * 128], hT[:, k, 0:N]) for k in range(KC)],
                           reads=[skey] + hk, writes=[bkey])
                        i2 = si % 2
                        si += 1
                        S.op(act, lambda: A.activation(out=sqt[i2][:, 0:N], in_=bank[:, 0:N], func=AF.Square),
                             reads=[bkey], writes=[("sqt", i2)])
                        if pend and not dbg.get('no_s2'):
                            qk_stage2(pend.pop(0))
                        pend.append(dict(kind=kind, c=c, bank=bank, bkey=bkey, i2=i2))
                    if col0 == KO:
                        for bi in range(nb):
                            bank, bkey = bank_alloc()
                            mm(bank[:, 0:256], [(hT[:, k, bi * 128:(bi + 1) * 128], slot[:, k, 256:512]) for k in range(KC)],
                               reads=[skey] + hkeys("hT", nb, [bi]), writes=[bkey])
                            S.op(act, lambda: A.copy(out=Vd[cb][:, bi, :, 0:64],
                                                     in_=bank[:, 0:256].rearrange("p (g d) -> p g d", d=64)),
                                 reads=[bkey], writes=[("Vd", cb, bi, 0)])
                            S.op(dve, lambda: V.tensor_copy(out=Vd[cb][:, bi, :, 64:128],
                                                            in_=bank[:, 0:256].rearrange("p (g d) -> p g d", d=64)),
                                 reads=[bkey], writes=[("Vd", cb, bi, 1)])
                while pend and not dbg.get('no_s2'):
                    qk_stage2(pend.pop(0))

                if dbg.get('m1_steps', 9) < 4:
                    continue
                pi = 0
                ri = 0
                bi_first = None
                for bi, b in enumerate(bl):
                    if b == b0:
                        continue
                    if bi_first is None:
                        bi_first = bi
                    for g in range(4):
                        kbs = []
                        if bi > 0:
                            kbs.append(("prev", cb, bi - 1, bl[bi - 1]))
                        else:
                            kbs.append(("prev", pb, nb_prev - 1, b - 1))
                        kbs.append(("cur", cb, bi, b))
                        pts = []
                        for kind, buf, sl, babs in kbs:
                            bank, bkey = bank_alloc()

                            def smm():
                                inst = None
                                for j in range(4):
                                    par, cq = j % 2, 2 * g + j // 2
                                    inst = T.matmul(bank[:, j * 128:(j + 1) * 128], lhsT=kT[buf][par][:, g, sl, :],
                                                    rhs=qT[:, cq, bi * 128:(bi + 1) * 128], start=True, stop=True)
                                return inst
                            S.op(pe, smm, reads=[("kT", buf, 0, g), ("kT", buf, 1, g), ("qT", 2 * g), ("qT", 2 * g + 1)],
                                 writes=[bkey])
                            i2 = pi % 2
                            i4 = pi % 4
                            pi += 1
                            if kind == "cur":
                                S.op(dve, lambda: V.tensor_tensor(out=tS[i2][:], in0=bank[:, :],
                                                                  in1=bcur[:, g * 512:(g + 1) * 512], op=ALU.add),
                                     reads=[bkey, "bcur"], writes=[("tS", i2)])
                            elif babs < 2:
                                S.op(dve, lambda: V.scalar_tensor_tensor(out=tS[i2][:], in0=bank[:, :], scalar=flag[:, 1:2],
                                                                         in1=bprev[:, g * 512:(g + 1) * 512],
                                                                         op0=ALU.add, op1=ALU.add),
                                     reads=[bkey, "bprev", "flag"], writes=[("tS", i2)])
                            else:
                                S.op(dve, lambda: V.tensor_tensor(out=tS[i2][:], in0=bank[:, :],
                                                                  in1=bprev[:, g * 512:(g + 1) * 512], op=ALU.add),
                                     reads=[bkey, "bprev"], writes=[("tS", i2)])
                            S.op(act, lambda: A.activation(out=PT[i4][:], in_=tS[i2][:], func=AF.Exp),
                                 reads=[("tS", i2)], writes=[("PT", i4)])
                            pts.append((i4, buf, sl))
                        bankn, bkn = bank_alloc()
                        bankd, bkd = bank_alloc()
                        mm(bankn[:, :], [(Vd[buf][:, sl, g, :], PT[i4][:]) for (i4, buf, sl) in pts],
                           reads=[("PT", p[0]) for p in pts] + [("Vd", p[1], p[2], h_) for p in pts for h_ in range(2)],
                           writes=[bkn])
                        mm(bankd[:, :], [(ones_bf[:], PT[i4][:]) for (i4, buf, sl) in pts],
                           reads=[("PT", p[0]) for p in pts] + ["ones"], writes=[bkd])
                        r2 = ri % 2
                        ri += 1

                        def addsink():
                            inst = None
                            for j in range(4):
                                h = 4 * g + j
                                inst = V.tensor_scalar(out=rc[r2][:, j * 128:(j + 1) * 128],
                                                       in0=bankd[:, j * 128:(j + 1) * 128],
                                                       scalar1=esink[:, l, h:h + 1], scalar2=None, op0=ALU.add)
                            return inst
                        S.op(dve, addsink, reads=[bkd] + ck, writes=[("rc", r2)])
                        S.op(dve, lambda: V.reciprocal(out=rc[r2][:], in_=rc[r2][:]), writes=[("rc", r2)])
                        for par in range(2):
                            ps_ = slice(par * 64, par * 64 + 64)
                            o = aT[ps_, 2 * g:2 * g + 2, bi * 128:(bi + 1) * 128]
                            i0 = bankn[ps_, :].rearrange("p (j q) -> p j q", q=128)[:, par::2, :]
                            i1 = rc[r2][ps_, :].rearrange("p (j q) -> p j q", q=128)[:, par::2, :]
                            S.op(dve, lambda: V.tensor_tensor(out=o, in0=i0, in1=i1, op=ALU.mult),
                                 reads=[bkn, ("rc", r2)], writes=[("aT", bi, g, par)])

                if dbg.get('m1_steps', 9) < 5:
                    continue
                for u_ in range(2):
                    slot, skey = ring.load(lambda s: [
                        (s[:, :, :], w_in[l, :, BO + u_ * 512:BO + (u_ + 1) * 512].rearrange("(k p) n -> p k n", p=128))])
                    for ci in range(4):
                        c = u_ * 4 + ci
                        bank, bkey = bank_alloc()
                        mm(bank[:, 0:N], [(slot[:, k, ci * 128:(ci + 1) * 128], hT[:, k, 0:N]) for k in range(KC)],
                           reads=[skey] + hk, writes=[bkey])
                        S.op(act, lambda: A.copy(out=Bt[:, c, 0:N], in_=bank[:, 0:N]), reads=[bkey], writes=[("Bt", c)])
                nh = sum(1 for b in bl if b < 2) * 128
                for u_ in range(4):
                    slot, skey = ring.load(lambda s: [
                        (s[:, :, 0:256], w_in[l, :, CO + u_ * 256:CO + (u_ + 1) * 256].rearrange("(k p) n -> p k n", p=128)),
                        (s[:, :, 256:512], w_in[l, :, XO + u_ * 256:XO + (u_ + 1) * 256].rearrange("(k p) n -> p k n", p=128))])
                    for ci in range(2):
                        c = u_ * 2 + ci
                        i2 = c % 2
                        bank, bkey = bank_alloc()
                        mm(bank[:, 0:N], [(slot[:, k, ci * 128:(ci + 1) * 128], hT[:, k, 0:N]) for k in range(KC)],
                           reads=[skey] + hk, writes=[bkey])
                        S.op(act, lambda: A.copy(out=Ct[i2][:, 0:N], in_=bank[:, 0:N]), reads=[bkey], writes=[("Ct", i2)])
                        bank2, bkey2 = bank_alloc()
                        mm(bank2[:, 0:N], [(slot[:, k, 256 + ci * 128:256 + (ci + 1) * 128], hT[:, k, 0:N]) for k in range(KC)],
                           reads=[skey] + hk, writes=[bkey2])

                        def mku():
                            inst = V.tensor_copy(out=ut[i2][:, 0:2], in_=ucar[pb][:, c, :])
                            if nh:
                                inst = V.scalar_tensor_tensor(out=ut[i2][:, 2:2 + nh], in0=bank2[:, 0:nh],
                                                              scalar=flag[:, 0:1], in1=Ct[i2][:, 0:nh],
                                                              op0=ALU.mult, op1=ALU.mult)
                            if nh < N:
                                inst = V.tensor_tensor(out=ut[i2][:, 2 + nh:2 + N], in0=bank2[:, nh:N],
                                                       in1=Ct[i2][:, nh:N], op=ALU.mult)
                            return inst
                        S.op(dve, mku, reads=[bkey2, ("Ct", i2), ("ucar", pb, c), "flag"], writes=[("ut", i2)])
                        S.op(dve, lambda: V.tensor_copy(out=ucar[cb][:, c, :], in_=ut[i2][:, N:N + 2]),
                             reads=[("ut", i2)], writes=[("ucar", cb, c)])
                        S.op(dve, lambda: V.tensor_scalar(out=yt[i2][:, 0:N], in0=ut[i2][:, 2:2 + N],
                                                          scalar1=convw[:, l, 2, c:c + 1], scalar2=None, op0=ALU.mult),
                             reads=[("ut", i2)] + ck, writes=[("yt", i2)])
                        S.op(dve, lambda: V.scalar_tensor_tensor(out=yt[i2][:, 0:N], in0=ut[i2][:, 1:1 + N],
                                                                 scalar=convw[:, l, 1, c:c + 1], in1=yt[i2][:, 0:N],
                                                                 op0=ALU.mult, op1=ALU.add),
                             reads=[("ut", i2)], writes=[("yt", i2)])
                        S.op(dve, lambda: V.scalar_tensor_tensor(out=yt[i2][:, 0:N], in0=ut[i2][:, 0:N],
                                                                 scalar=convw[:, l, 0, c:c + 1], in1=yt[i2][:, 0:N],
                                                                 op0=ALU.mult, op1=ALU.add),
                             reads=[("ut", i2)], writes=[("yt", i2)])
                        S.op(dve, lambda: V.tensor_tensor(out=cT[:, c, 0:N], in0=yt[i2][:, 0:N], in1=Bt[:, c, 0:N],
                                                          op=ALU.mult),
                             reads=[("yt", i2), ("Bt", c)], writes=[("cT", c)])

                if bi_first is not None:
                    c0_ = bi_first * 128
                    S.dma(aT_d[:, :, tok0 + c0_:tok0 + N].rearrange("k p t -> p k t"), aT[:, :, c0_:N],
                          reads=[("aT", bi, g, par) for bi in range(bi_first, nb) for g in range(4) for par in range(2)],
                          writes=[("aTd", b) for b in bl[bi_first:]])
                    S.dma(cT_d[:, :, tok0 + c0_:tok0 + N].rearrange("k p t -> p k t"), cT[:, :, c0_:N],
                          reads=[("cT", c) for c in range(8)], writes=[("cTd", b) for b in bl[bi_first:]])
            S.barrier()

        if dbg.get("stop") == f"M1_{l}":
            break

        with contextlib.ExitStack() as pm:
            gbc = sb("gbc", [128, D], stack=pm)
            hT = sb("hT2", [128, KC, 512], BF16, stack=pm)
            aT = sb("aT2", [128, 8, 512], BF16, stack=pm)
            cT = sb("cT2", [128, 8, 512], BF16, stack=pm)
            mg = sb("mg", [128, KC, 512], BF16, stack=pm)
            xt = sb("xt", [128, 4, D], stack=pm)
            tA = [[sb(f"tA{br}_{i}", [128, 512], stack=pm) for i in range(2)] for br in range(2)]
            tB = [[sb(f"tB{br}_{i}", [128, 512], stack=pm) for i in range(2)] for br in range(2)]
            to = [sb(f"to{i}", [128, 512], stack=pm) for i in range(2)]
            S.dma(gbc[:], mod_d[l, 2 * D:3 * D].partition_broadcast(128), writes=["gbc"])
            for ti, bl in enumerate(tiles_from(b0 + 1)):
                nb = len(bl)
                N = nb * 128
                tok0 = bl[0] * 128
                S.dma(hT[:, :, 0:N], hT_d[:, :, tok0:tok0 + N].rearrange("k p t -> p k t"),
                      reads=[("hTd", b) for b in bl], writes=["hT2"])
                S.dma(aT[:, :, 0:N], aT_d[:, :, tok0:tok0 + N].rearrange("k p t -> p k t"),
                      reads=[("aTd", b) for b in bl], writes=["aT2"])
                S.dma(cT[:, :, 0:N], cT_d[:, :, tok0:tok0 + N].rearrange("k p t -> p k t"),
                      reads=[("cTd", b) for b in bl], writes=["cT2"])
                for bi, b in enumerate(bl):
                    S.dma(xt[:, bi, :], xsrc[b * 128:(b + 1) * 128, :], reads=[("xs", b)],
                          writes=[("xt", bi, cc) for cc in range(4)])
                ci_ = 0
                for j2 in range(8):
                    slots = []
                    for (gofs, wbr) in ((GAO, w_ab), (GCO, w_cb)):
                        slots.append(ring.load(lambda s: [
                            (s[:, :, 0:256], w_in[l, :, gofs + j2 * 256:gofs + (j2 + 1) * 256].rearrange("(k p) n -> p k n", p=128)),
                            (s[:, 0:8, 256:512], wbr[l, :, j2 * 256:(j2 + 1) * 256].rearrange("(k p) n -> p k n", p=128))]))
                    for jj in range(2):
                        j = 2 * j2 + jj
                        i2 = ci_ % 2
                        ci_ += 1
                        for br, (slot, skey), src, skn in ((0, slots[0], aT, "aT2"), (1, slots[1], cT, "cT2")):
                            bank, bkey = bank_alloc()
                            mm(bank[:, 0:N], [(slot[:, k, jj * 128:(jj + 1) * 128], hT[:, k, 0:N]) for k in range(KC)],
                               reads=[skey, "hT2"], writes=[bkey])
                            bank2, bkey2 = bank_alloc()
                            mm(bank2[:, 0:N], [(slot[:, k, 256 + jj * 128:256 + (jj + 1) * 128], src[:, k, 0:N]) for k in range(8)],
                               reads=[skey, skn], writes=[bkey2])
                            S.op(act, lambda: A.activation(out=tA[br][i2][:, 0:N], in_=bank[:, 0:N], func=AF.Sigmoid),
                                 reads=[bkey], writes=[("tA", br, i2)])
                            S.op(dve, lambda: V.tensor_tensor(out=tB[br][i2][:, 0:N], in0=bank2[:, 0:N],
                                                              in1=tA[br][i2][:, 0:N], op=ALU.mult),
                                 reads=[bkey2, ("tA", br, i2)], writes=[("tB", br, i2)])
                        S.op(dve, lambda: V.tensor_tensor(out=mg[:, j, 0:N], in0=tB[0][i2][:, 0:N], in1=tB[1][i2][:, 0:N],
                                                          op=ALU.add),
                             reads=[("tB", 0, i2), ("tB", 1, i2)], writes=[("mg", j)])
                oi = 0
                for cc in range(4):
                    slot, skey = ring.load(lambda s: [
                        (s[:, :, :], w_out[l, :, cc * 512:(cc + 1) * 512].rearrange("(k p) n -> p k n", p=128))])
                    for bi in range(nb):
                        bank, bkey = bank_alloc()
                        mm(bank[:, :], [(mg[:, k, bi * 128:(bi + 1) * 128], slot[:, k, :]) for k in range(KC)],
                           reads=[skey] + [("mg", j) for j in range(KC)], writes=[bkey])
                        i2 = oi % 2
                        oi += 1
                        S.op(dve, lambda: V.tensor_tensor(out=to[i2][:, :], in0=bank[:, :],
                                                          in1=gbc[:, cc * 512:(cc + 1) * 512], op=ALU.mult),
                             reads=[bkey, "gbc"], writes=[("to", i2)])
                        S.op(dve, lambda: V.tensor_tensor(out=xt[:, bi, cc * 512:(cc + 1) * 512],
                                                          in0=xt[:, bi, cc * 512:(cc + 1) * 512], in1=to[i2][:, :],
                                                          op=ALU.add),
                             reads=[("to", i2)], writes=[("xt", bi, cc)])
                for bi, b in enumerate(bl):
                    S.dma(xs[b * 128:(b + 1) * 128, :], xt[:, bi, :], reads=[("xt", bi, cc) for cc in range(4)],
                          writes=[("xs", b)])
            S.barrier()
        xsrc = xs

        if dbg.get("stop") == f"M2_{l}":
            break

        with contextlib.ExitStack() as pf:
            gbc = sb("gbcf", [128, D], stack=pf)
            xt = sb("xtf", [128, 4, D], stack=pf)
            facc = sb("facc", [128, 4, D], stack=pf)
            hT = sb("hTf", [128, KC, 512], BF16, stack=pf)
            xn = sb("xnf", [128, D], stack=pf)
            ss = sb("ssf", [128, 4], stack=pf)
            actb = [sb(f"actb{i}", [128, 8, 512], BF16, stack=pf) for i in range(2)]
            ts_ = [sb(f"tsf{i}", [128, 512], stack=pf) for i in range(2)]
            gates = sb("gates", [128, 4, NEXP], stack=pf)
            rt = sb("rt", [128, 8, NEXP], stack=pf)
            S.dma(gbc[:], mod_d[l, 5 * D:6 * D].partition_broadcast(128), writes=["gbc"])
            is_moe = (l % 2 == 1)
            FFC = (MOE_FF if is_moe else D_FF) // 128
            ffw = MOE_FF if is_moe else D_FF
            n_exp = NEXP if is_moe else 1
            for ti, bl in enumerate(tiles_from(b0 + 1)):
                nb = len(bl)
                N = nb * 128
                hk = hkeys("hTf", nb)
                for bi, b in enumerate(bl):
                    S.dma(xt[:, bi, :], xs[b * 128:(b + 1) * 128, :], reads=[("xs", b)], writes=[("xtf", bi)])
                for bi, b in enumerate(bl):
                    norm_transpose(l, 1, xt[:, bi, :], [("xtf", bi)], hT, "hTf", bi, xn, ss)
                if is_moe:
                    for bi in range(nb):
                        bank, bkey = bank_alloc()
                        mm(bank[:, 0:NEXP], [(hT[:, k, bi * 128:(bi + 1) * 128], wr_sb[:, k, :]) for k in range(KC)],
                           reads=["wr"] + hkeys("hTf", nb, [bi]), writes=[bkey])
                        lg, eq1, lg2, eq2 = rt[:, 0, :], rt[:, 1, :], rt[:, 2, :], rt[:, 3, :]
                        sc = rt[:, 4, :]
                        R = ["rt"]
                        S.op(dve, lambda: V.tensor_copy(out=lg, in_=bank[:, 0:NEXP]), reads=[bkey], writes=R)
                        S.op(dve, lambda: V.reduce_max(out=sc[:, 0:1], in_=lg, axis=AX.X), writes=R)
                        S.op(dve, lambda: V.tensor_scalar(out=eq1, in0=lg, scalar1=sc[:, 0:1], scalar2=None,
                                                          op0=ALU.is_equal), writes=R)
                        S.op(dve, lambda: V.scalar_tensor_tensor(out=lg2, in0=eq1, scalar=-1e30, in1=lg,
                                                                 op0=ALU.mult, op1=ALU.add), writes=R)
                        S.op(dve, lambda: V.reduce_max(out=sc[:, 1:2], in_=lg2, axis=AX.X), writes=R)
                        S.op(dve, lambda: V.tensor_scalar(out=eq2, in0=lg2, scalar1=sc[:, 1:2], scalar2=None,
                                                          op0=ALU.is_equal), writes=R)
                        S.op(dve, lambda: V.tensor_tensor(out=sc[:, 2:3], in0=sc[:, 1:2], in1=sc[:, 0:1],
                                                          op=ALU.subtract), writes=R)
                        S.op(act, lambda: A.activation(out=sc[:, 3:4], in_=sc[:, 2:3], func=AF.Exp), writes=R)
                        S.op(dve, lambda: V.tensor_scalar(out=sc[:, 4:5], in0=sc[:, 3:4], scalar1=1.0, scalar2=None,
                                                          op0=ALU.add), writes=R)
                        S.op(dve, lambda: V.reciprocal(out=sc[:, 5:6], in_=sc[:, 4:5]), writes=R)
                        S.op(dve, lambda: V.tensor_tensor(out=sc[:, 6:7], in0=sc[:, 3:4], in1=sc[:, 5:6], op=ALU.mult),
                             writes=R)
                        S.op(dve, lambda: V.tensor_scalar(out=eq1, in0=eq1, scalar1=sc[:, 5:6], scalar2=None,
                                                          op0=ALU.mult), writes=R)
                        S.op(dve, lambda: V.scalar_tensor_tensor(out=gates[:, bi, :], in0=eq2, scalar=sc[:, 6:7],
                                                                 in1=eq1, op0=ALU.mult, op1=ALU.add),
                             reads=R, writes=[("gates", bi)])

                groups = []
                for e_ in range(n_exp):
                    c0 = 0
                    while c0 < FFC:
                        gsz = min(8, FFC - c0)
                        groups.append((e_, c0, gsz))
                        c0 += gsz

                def gate_up(gi):
                    e_, c0, gsz = groups[gi]
                    ab = actb[gi % 2]
                    wgu = moe_gu[0, e_] if is_moe else ffn_gu[0]
                    for s0 in range(0, gsz, 4):
                        ns = min(4, gsz - s0)
                        col = (c0 + s0) * 128
                        sg, kg = ring.load(lambda s: [
                            (s[:, :, 0:ns * 128], wgu[:, col:col + ns * 128].rearrange("(k p) n -> p k n", p=128))])
                        su, ku = ring.load(lambda s: [
                            (s[:, :, 0:ns * 128], wgu[:, ffw + col:ffw + col + ns * 128].rearrange("(k p) n -> p k n", p=128))])
                        for ci in range(ns):
                            i2 = (s0 + ci) % 2
                            bank, bkey = bank_alloc()
                            mm(bank[:, 0:N], [(sg[:, k, ci * 128:(ci + 1) * 128], hT[:, k, 0:N]) for k in range(KC)],
                               reads=[kg] + hk, writes=[bkey])
                            bank2, bkey2 = bank_alloc()
                            mm(bank2[:, 0:N], [(su[:, k, ci * 128:(ci + 1) * 128], hT[:, k, 0:N]) for k in range(KC)],
                               reads=[ku] + hk, writes=[bkey2])
                            S.op(act, lambda: A.activation(out=ts_[i2][:, 0:N], in_=bank[:, 0:N], func=AF.Silu),
                                 reads=[bkey], writes=[("ts", i2)])
                            S.op(dve, lambda: V.tensor_tensor(out=ab[:, s0 + ci, 0:N], in0=bank2[:, 0:N],
                                                              in1=ts_[i2][:, 0:N], op=ALU.mult),
                                 reads=[bkey2, ("ts", i2)], writes=[("actb", gi % 2, s0 + ci)])

                def down(gi):
                    e_, c0, gsz = groups[gi]
                    ab = actb[gi % 2]
                    wdn = moe_dn[0, e_] if is_moe else ffn_dn[0]
                    akeys = [("actb", gi % 2, ci) for ci in range(gsz)]
                    for hc in range(2):
                        sd_, kd = ring.load(lambda s: [
                            (s[:].rearrange("p k n -> p (k n)")[:, 0:gsz * 1024].rearrange("p (k n) -> p k n", n=1024),
                             wdn[c0 * 128:(c0 + gsz) * 128, hc * 1024:(hc + 1) * 1024].rearrange("(k p) n -> p k n", p=128))])
                        sd = sd_[:].rearrange("p k n -> p (k n)")[:, 0:gsz * 1024].rearrange("p (k n) -> p k n", n=1024)
                        for hh in range(2):
                            cq = hc * 2 + hh
                            cs = slice(cq * 512, (cq + 1) * 512)
                            for bi in range(nb):
                                bank, bkey = bank_alloc()
                                mm(bank[:, :],
                                   [(ab[:, k, bi * 128:(bi + 1) * 128], sd[:, k, hh * 512:(hh + 1) * 512]) for k in range(gsz)],
                                   reads=[kd] + akeys, writes=[bkey])
                                fa = facc[:, bi, cs]
                                fk = ("facc", bi, cq)
                                if is_moe:
                                    gsc = gates[:, bi, e_:e_ + 1]
                                    if gi == 0:
                                        S.op(dve, lambda: V.tensor_scalar(out=fa, in0=bank[:, :], scalar1=gsc, scalar2=None,
                                                                          op0=ALU.mult),
                                             reads=[bkey, ("gates", bi)], writes=[fk])
                                    else:
                                        S.op(dve, lambda: V.scalar_tensor_tensor(out=fa, in0=bank[:, :], scalar=gsc, in1=fa,
                                                                                 op0=ALU.mult, op1=ALU.add),
                                             reads=[bkey, ("gates", bi)], writes=[fk])
                                else:
                                    if gi == 0:
                                        S.op(dve, lambda: V.tensor_copy(out=fa, in_=bank[:, :]), reads=[bkey], writes=[fk])
                                    else:
                                        S.op(dve, lambda: V.tensor_tensor(out=fa, in0=bank[:, :], in1=fa, op=ALU.add),
                                             reads=[bkey], writes=[fk])

                for gi in range(len(groups)):
                    gate_up(gi)
                    if gi > 0:
                        down(gi - 1)
                down(len(groups) - 1)

                for bi, b in enumerate(bl):
                    fks = [("facc", bi, cq) for cq in range(4)]
                    S.op(dve, lambda: V.tensor_tensor(out=facc[:, bi, :], in0=facc[:, bi, :], in1=gbc[:, :], op=ALU.mult),
                         reads=["gbc"], writes=fks)
                    S.op(dve, lambda: V.tensor_tensor(out=xt[:, bi, :], in0=xt[:, bi, :], in1=facc[:, bi, :], op=ALU.add),
                         reads=fks, writes=[("xtf", bi)])
                    if l == DEPTH - 1:
                        S.dma(y[(b - 2) * 128:(b - 1) * 128, :], xt[:, bi, :], reads=[("xtf", bi)], writes=[("y", b)])
                    else:
                        S.dma(xs[b * 128:(b + 1) * 128, :], xt[:, bi, :], reads=[("xtf", bi)], writes=[("xs", b)])
            S.barrier()

        if dbg.get("stop") == f"F_{l}":
            break

    S.barrier()
    es.close()
    return nc


def _alibi_tables():
    h = np.arange(1, 17, dtype=np.float32)
    slopes = (2.0 ** (-8.0 * h / 16.0)).astype(np.float32)
    k = np.arange(128)[:, None]
    q = np.arange(128)[None, :]
    cur = np.full((128, 16, 128), NEG, np.float32)
    prev = np.full((128, 16, 128), NEG, np.float32)
    dc = (q - k).astype(np.float32)
    dp = (q - k + 128).astype(np.float32)
    for hh in range(16):
        cur[:, hh, :] = np.where(dc >= 0, -slopes[hh] * dc, NEG)
        prev[:, hh, :] = np.where(dp < 128, -slopes[hh] * dp, NEG)
    return cur.reshape(128, 2048), prev.reshape(128, 2048)


def make_in_maps(inputs, cores=range(8)):
    x = np.asarray(inputs["x"], np.float32)
    c = np.asarray(inputs["c"], np.float32)
    cur, prev = _alibi_tables()
    ident = np.eye(128, dtype=np.float32)
    shared = {k: np.ascontiguousarray(np.asarray(v, np.float32)) for k, v in inputs.items() if k not in ("x", "c")}
    maps = []
    for core in cores:
        b, half = core // 2, core % 2
        xh = np.zeros((NTOK, D), np.float32)
        if half == 0:
            xh[256:] = x[b, 0:2048]
        else:
            xh[:] = x[b, 2048 - 256:4096]
        fl = np.zeros((128, 2), np.float32)
        fl[:, 0] = 1.0 if half else 0.0
        fl[:, 1] = 0.0 if half else NEG
        m = dict(shared)
        m.update({"xh": xh, "ccol": np.ascontiguousarray(c[b].reshape(KC, 128).T), "flag": fl, "ident": ident,
                  "biascur": cur, "biasprev": prev})
        maps.append(m)
    return maps


_NC_CACHE = {}


def kernel(**inputs):
    if "nc" not in _NC_CACHE:
        _NC_CACHE["nc"] = build()
    nc = _NC_CACHE["nc"]
    maps = make_in_maps(inputs)
    res = run_bass_kernel_spmd(nc, maps, core_ids=list(range(8)))
    out = np.empty((4, 4096, D), np.float32)
    for core in range(8):
        b, half = core // 2, core % 2
        out[b, half * 2048:(half + 1) * 2048] = res.results[core]["y"]
    return out
```
